# Optimizing a Trainium2 kernel written in Bass

```python
import math
import jax, jax.numpy as jnp
from jax import lax
import numpy as np

D_MODEL = 1024
BATCH = 4
SEQ = 4096
DEPTH = 4

HEAD_DIM = 64
ATTN_SCALE = HEAD_DIM ** -0.5
SWA_HEADS = 8
SWA_KV_HEADS = 2
SWA_WINDOW = 128
SWA_Q = SWA_HEADS * HEAD_DIM
SWA_KV = SWA_KV_HEADS * HEAD_DIM
SSM_WIDTH = 512
SSM_GROUP = 16
SSM_GROUPS = SSM_WIDTH // SSM_GROUP
SSM_STATE = 64
DT_MIN = 0.001
DT_MAX = 0.1
MOBA_HEADS = 8
MOBA_KV_HEADS = 2
MOBA_BLOCK = 256
MOBA_TOPK = 3
MOBA_Q_CHUNK = 64
MOBA_Q = MOBA_HEADS * HEAD_DIM
MOBA_KV = MOBA_KV_HEADS * HEAD_DIM
N_BRANCH = 3
D_FF = 4 * D_MODEL
NORM_EPS = 1e-6
ALIBI_MAX_BIAS = 8.0
IN_SIZES = (SWA_Q, SWA_KV, SWA_KV, SSM_WIDTH, MOBA_Q, MOBA_KV, MOBA_KV, N_BRANCH * D_MODEL)
D_IN = sum(IN_SIZES)

kernel_name = "hybrid_swa_s5_moba_gated"


def rms_norm(x, gain):
    xf = x.astype(jnp.float32)
    y = xf * lax.rsqrt(jnp.mean(xf * xf, axis=-1, keepdims=True) + NORM_EPS)
    return (y * gain.astype(jnp.float32)).astype(x.dtype)


def alibi_slopes():
    n = SWA_HEADS + MOBA_HEADS
    return jnp.asarray(2.0 ** (-ALIBI_MAX_BIAS * np.arange(1, n + 1) / n), jnp.float32)


def split_in(proj):
    outs, start = [], 0
    for size in IN_SIZES:
        outs.append(proj[..., start:start + size])
        start += size
    return outs


def swa_attention(q, k, v, sinks, slopes):
    B, S, Hq, d = q.shape
    Hkv = k.shape[2]
    G = Hq // Hkv
    W = SWA_WINDOW
    nb = S // W
    qb = q.reshape(B, nb, W, Hkv, G, d)
    kp = jnp.pad(k, ((0, 0), (W, 0), (0, 0), (0, 0))).reshape(B, nb + 1, W, Hkv, d)
    vp = jnp.pad(v, ((0, 0), (W, 0), (0, 0), (0, 0))).reshape(B, nb + 1, W, Hkv, d)
    kb = jnp.concatenate([kp[:, :-1], kp[:, 1:]], axis=2)
    vb = jnp.concatenate([vp[:, :-1], vp[:, 1:]], axis=2)
    s = jnp.einsum('bnqhgd,bnkhd->bnhgqk', qb, kb).astype(jnp.float32) * ATTN_SCALE
    t_rel = jnp.arange(W)[:, None]
    s_rel = jnp.arange(2 * W)[None, :] - W
    dist = t_rel - s_rel
    band = (dist >= 0) & (dist < W)
    mask = band[None] & ((jnp.arange(nb)[:, None, None] * W + s_rel[None]) >= 0)
    bias = -slopes.reshape(Hkv, G)[:, :, None, None] * dist.astype(jnp.float32)[None, None]
    s = jnp.where(mask[None, :, None, None], s + bias[None, None], -jnp.inf)
    sink = jnp.broadcast_to(sinks.astype(jnp.float32).reshape(Hkv, G)[None, None, :, :, None, None],
                            s.shape[:-1] + (1,))
    p = jax.nn.softmax(jnp.concatenate([s, sink], axis=-1), axis=-1)[..., :-1].astype(v.dtype)
    o = jnp.einsum('bnhgqk,bnkhd->bnqhgd', p, vb)
    return o.reshape(B, S, Hq * d)


def moba_attention(q, k, v, slopes):
    B, S, Hq, d = q.shape
    Hkv = k.shape[2]
    G = Hq // Hkv
    L = MOBA_BLOCK
    Qc = MOBA_Q_CHUNK
    Sp = -(-S // L) * L
    nblk = Sp // L
    topk = min(MOBA_TOPK, nblk)
    pad = ((0, 0), (0, Sp - S), (0, 0), (0, 0))
    q, k, v = jnp.pad(q, pad), jnp.pad(k, pad), jnp.pad(v, pad)
    k_blocks = jnp.repeat(k, G, axis=2).transpose(0, 2, 1, 3).reshape(B, Hq, nblk, L, d)
    v_blocks = jnp.repeat(v, G, axis=2).transpose(0, 2, 1, 3).reshape(B, Hq, nblk, L, d)
    k_mean = jnp.mean(k_blocks.astype(jnp.float32), axis=3)
    nq = Sp // Qc
    q_chunks = q.transpose(0, 2, 1, 3).reshape(B, Hq, nq, Qc, d).transpose(2, 0, 1, 3, 4)
    b_idx = jnp.arange(B)[:, None, None, None]
    h_idx = jnp.arange(Hq)[None, :, None, None]
    blk_ids = jnp.arange(nblk)
    rank_ids = jnp.arange(topk)
    offs = jnp.arange(L)

    def one_chunk(args):
        qi, ci = args
        t = ci * Qc + jnp.arange(Qc)
        j = (ci * Qc) // L
        gate = jnp.einsum('bhqd,bhnd->bhqn', qi.astype(jnp.float32), k_mean)
        gate = jnp.where(blk_ids < j, gate, -jnp.inf)
        _, sel = lax.top_k(gate, topk)
        valid = rank_ids < j
        k_sel = k_blocks[b_idx, h_idx, sel]
        v_sel = v_blocks[b_idx, h_idx, sel]
        s_sel = jnp.einsum('bhqd,bhqrkd->bhqrk', qi, k_sel).astype(jnp.float32) * ATTN_SCALE
        dist_sel = (t[None, None, :, None, None] - (sel[..., None] * L + offs)).astype(jnp.float32)
        s_sel = s_sel - slopes[None, :, None, None, None] * dist_sel
        s_sel = jnp.where(valid[None, None, None, :, None], s_sel, -jnp.inf)
        k_own = lax.dynamic_slice_in_dim(k_blocks, j, 1, axis=2)[:, :, 0]
        v_own = lax.dynamic_slice_in_dim(v_blocks, j, 1, axis=2)[:, :, 0]
        s_own = jnp.einsum('bhqd,bhkd->bhqk', qi, k_own).astype(jnp.float32) * ATTN_SCALE
        dist_own = t[:, None] - (j * L + offs)[None, :]
        s_own = s_own - slopes[None, :, None, None] * dist_own.astype(jnp.float32)[None, None]
        s_own = jnp.where((dist_own >= 0)[None, None], s_own, -jnp.inf)
        logits = jnp.concatenate([s_sel.reshape(B, Hq, Qc, topk * L), s_own], axis=-1)
        p = jax.nn.softmax(logits, axis=-1).astype(v_blocks.dtype)
        p_sel = p[..., :topk * L].reshape(B, Hq, Qc, topk, L)
        o = (jnp.einsum('bhqrk,bhqrkd->bhqd', p_sel, v_sel)
             + jnp.einsum('bhqk,bhkd->bhqd', p[..., topk * L:], v_own))
        return o

    o = lax.map(one_chunk, (q_chunks, jnp.arange(nq)))
    o = o.transpose(1, 0, 3, 2, 4).reshape(B, Sp, Hq * d)
    return o[:, :S]


def s5_ssm(u, lam_re, lam_im, log_step, b_re, b_im, c_re, c_im, d_skip):
    Bsz, S, _ = u.shape
    f32 = jnp.float32
    uf = u.astype(f32).reshape(Bsz, S, SSM_GROUPS, SSM_GROUP)
    lr, li = lam_re.astype(f32), lam_im.astype(f32)
    dt = jnp.exp(log_step.astype(f32))[:, None]
    mag = jnp.exp(lr * dt)
    ab_re, ab_im = mag * jnp.cos(li * dt), mag * jnp.sin(li * dt)
    den = lr * lr + li * li
    nr, ni = ab_re - 1.0, ab_im
    f_re, f_im = (nr * lr + ni * li) / den, (ni * lr - nr * li) / den
    br, bi = b_re.astype(f32), b_im.astype(f32)
    bb_re = f_re[..., None] * br - f_im[..., None] * bi
    bb_im = f_re[..., None] * bi + f_im[..., None] * br
    x_re = jnp.einsum('bsgh,gph->bsgp', uf, bb_re)
    x_im = jnp.einsum('bsgh,gph->bsgp', uf, bb_im)
    a_re = jnp.broadcast_to(ab_re, x_re.shape)
    a_im = jnp.broadcast_to(ab_im, x_im.shape)

    def combine(e1, e2):
        a1r, a1i, b1r, b1i = e1
        a2r, a2i, b2r, b2i = e2
        return (a2r * a1r - a2i * a1i, a2r * a1i + a2i * a1r,
                a2r * b1r - a2i * b1i + b2r, a2r * b1i + a2i * b1r + b2i)

    _, _, h_re, h_im = lax.associative_scan(combine, (a_re, a_im, x_re, x_im), axis=1)
    y = (jnp.einsum('bsgp,ghp->bsgh', h_re, c_re.astype(f32))
         - jnp.einsum('bsgp,ghp->bsgh', h_im, c_im.astype(f32))
         + d_skip.astype(f32).reshape(SSM_GROUPS, SSM_GROUP) * uf)
    return y.reshape(Bsz, S, SSM_WIDTH)


def setup_inputs(seed: int = 0) -> dict:
    key = jax.random.key(seed)
    ks = jax.random.split(key, 24)
    f32 = jnp.float32
    nrm = lambda k, shape, scale: jax.random.normal(k, shape, f32) * scale
    x = jax.random.normal(ks[0], (BATCH, SEQ, D_MODEL), f32)
    norm_mix = 1.0 + nrm(ks[1], (DEPTH, D_MODEL), 0.02)
    w_in = nrm(ks[2], (DEPTH, D_MODEL, D_IN), D_MODEL ** -0.5)
    sinks = nrm(ks[3], (DEPTH, SWA_HEADS), 0.5)
    lam_re = -0.5 + nrm(ks[4], (DEPTH, SSM_GROUPS, SSM_STATE), 0.01)
    lam_im = (math.pi * jnp.arange(SSM_STATE, dtype=f32))[None, None, :] + nrm(ks[5], (DEPTH, SSM_GROUPS, SSM_STATE), 0.01)
    log_step = math.log(DT_MIN) + jax.random.uniform(ks[6], (DEPTH, SSM_GROUPS), f32) * (math.log(DT_MAX) - math.log(DT_MIN))
    b_re = nrm(ks[7], (DEPTH, SSM_GROUPS, SSM_STATE, SSM_GROUP), (2 * SSM_GROUP) ** -0.5)
    b_im = nrm(ks[8], (DEPTH, SSM_GROUPS, SSM_STATE, SSM_GROUP), (2 * SSM_GROUP) ** -0.5)
    c_re = nrm(ks[9], (DEPTH, SSM_GROUPS, SSM_GROUP, SSM_STATE), (2 * SSM_STATE) ** -0.5)
    c_im = nrm(ks[10], (DEPTH, SSM_GROUPS, SSM_GROUP, SSM_STATE), (2 * SSM_STATE) ** -0.5)
    d_skip = nrm(ks[11], (DEPTH, SSM_WIDTH), 1.0)
    w_glu = nrm(ks[12], (DEPTH, SSM_WIDTH, 2 * SSM_WIDTH), SSM_WIDTH ** -0.5)
    w_o_swa = nrm(ks[13], (DEPTH, SWA_Q, D_MODEL), SWA_Q ** -0.5)
    w_o_ssm = nrm(ks[14], (DEPTH, SSM_WIDTH, D_MODEL), SSM_WIDTH ** -0.5)
    w_o_moba = nrm(ks[15], (DEPTH, MOBA_Q, D_MODEL), MOBA_Q ** -0.5)
    w_out = nrm(ks[16], (DEPTH, D_MODEL, D_MODEL), D_MODEL ** -0.5)
    norm_ffn = 1.0 + nrm(ks[17], (DEPTH, D_MODEL), 0.02)
    w_ff1 = nrm(ks[18], (DEPTH, D_MODEL, D_FF), D_MODEL ** -0.5)
    w_ff2 = nrm(ks[19], (DEPTH, D_FF, D_MODEL), D_FF ** -0.5)
    norm_final = 1.0 + nrm(ks[20], (D_MODEL,), 0.02)
    return {"x": x, "norm_mix": norm_mix, "w_in": w_in, "sinks": sinks,
            "lam_re": lam_re, "lam_im": lam_im, "log_step": log_step,
            "b_re": b_re, "b_im": b_im, "c_re": c_re, "c_im": c_im, "d_skip": d_skip,
            "w_glu": w_glu, "w_o_swa": w_o_swa, "w_o_ssm": w_o_ssm, "w_o_moba": w_o_moba,
            "w_out": w_out, "norm_ffn": norm_ffn, "w_ff1": w_ff1, "w_ff2": w_ff2,
            "norm_final": norm_final}


def reference(x, norm_mix, w_in, sinks, lam_re, lam_im, log_step, b_re, b_im, c_re, c_im,
              d_skip, w_glu, w_o_swa, w_o_ssm, w_o_moba, w_out, norm_ffn, w_ff1, w_ff2,
              norm_final):
    B, S, _ = x.shape
    slopes = alibi_slopes()
    slopes_swa, slopes_moba = slopes[:SWA_HEADS], slopes[SWA_HEADS:]
    for l in range(DEPTH):
        h = rms_norm(x, norm_mix[l])
        qa, ka, va, u, qm, km, vm, gates = split_in(h @ w_in[l])
        oa = swa_attention(qa.reshape(B, S, SWA_HEADS, HEAD_DIM),
                           ka.reshape(B, S, SWA_KV_HEADS, HEAD_DIM),
                           va.reshape(B, S, SWA_KV_HEADS, HEAD_DIM), sinks[l], slopes_swa)
        ya = oa @ w_o_swa[l]
        ys = s5_ssm(u, lam_re[l], lam_im[l], log_step[l], b_re[l], b_im[l], c_re[l], c_im[l], d_skip[l])
        z = jax.nn.gelu(ys).astype(x.dtype) @ w_glu[l]
        yb = (z[..., :SSM_WIDTH] * jax.nn.sigmoid(z[..., SSM_WIDTH:])) @ w_o_ssm[l]
        om = moba_attention(qm.reshape(B, S, MOBA_HEADS, HEAD_DIM),
                            km.reshape(B, S, MOBA_KV_HEADS, HEAD_DIM),
                            vm.reshape(B, S, MOBA_KV_HEADS, HEAD_DIM), slopes_moba)
        yc = om @ w_o_moba[l]
        g = jax.nn.sigmoid(gates)
        mixed = (g[..., :D_MODEL] * ya + g[..., D_MODEL:2 * D_MODEL] * yb
                 + g[..., 2 * D_MODEL:] * yc)
        x = x + mixed @ w_out[l]
        h = rms_norm(x, norm_ffn[l])
        x = x + jnp.square(jax.nn.relu(h @ w_ff1[l])) @ w_ff2[l]
    return rms_norm(x, norm_final)
```

```python
import math
from contextlib import ExitStack

import numpy as np
import ml_dtypes

import concourse.bass as bass
import concourse.mybir as mybir
from concourse.bass_utils import run_bass_kernel_spmd

F32 = mybir.dt.float32
BF16 = mybir.dt.bfloat16
I32 = mybir.dt.int32
ALU = mybir.AluOpType
AF = mybir.ActivationFunctionType
AX = mybir.AxisListType

D = 1024
KD = 8
HD = 64
NH = 8
GT = 256
NTG = GT // 128
SSM_W = 512
SG = 32
SP = 64
SK = 8
CG = GT // SK
DFF = 4096
EPS = 1e-6
NEG = -240000.0
TOPK = 3

_sl = 2.0 ** (-8.0 * np.arange(1, 17) / 16.0)
SLOPES_SWA = _sl[:8].astype(np.float64)
SLOPES_MOBA = _sl[8:].astype(np.float64)


class Buf:
    __slots__ = ("name", "w", "r")

    def __init__(self, name=""):
        self.name = name
        self.w = None
        self.r = {}


class Ctx:
    def __init__(self, nc, es, ndsem=24):
        self.nc = nc
        self.eng = {"pe": nc.tensor, "act": nc.scalar, "dve": nc.vector, "pool": nc.gpsimd, "sp": nc.sync}
        self.sem = {}
        self.cnt = {}
        for e in self.eng:
            self.sem[e] = es.enter_context(nc.semaphore("s_" + e))
            self.cnt[e] = 0
        self.dsem = [es.enter_context(nc.semaphore(f"d_{i}")) for i in range(ndsem)]
        self.dcnt = [0] * ndsem
        half = ndsem // 2
        self.dpool = {"sp": list(range(0, half)), "pool": list(range(half, ndsem))}
        self.dnext = {"sp": 0, "pool": 0}
        self.waited = {e: {} for e in self.eng}
        self.nins = 0

    def new_epoch(self, es, tag):
        for e in self.eng:
            self.sem[e] = es.enter_context(self.nc.semaphore(f"s_{e}_{tag}"))
            self.cnt[e] = 0

    def _wait(self, e, evs):
        for (sem, val) in evs:
            if sem is self.sem[e] and e == "pe":
                continue
            k = id(sem)
            if self.waited[e].get(k, 0) >= val:
                continue
            self.waited[e][k] = val
            self.eng[e].wait_ge(sem, val)

    @staticmethod
    def _deps(reads, writes):
        evs = []
        for b in reads:
            if b.w is not None:
                evs.append(b.w)
        for b in writes:
            if b.w is not None:
                evs.append(b.w)
            evs.extend(b.r.values())
        return evs

    @staticmethod
    def _mark(ev, reads, writes):
        for b in reads:
            b.r[id(ev[0])] = ev
        for b in writes:
            b.w = ev
            b.r = {}

    def op(self, e, fn, reads=(), writes=()):
        self._wait(e, self._deps(reads, writes))
        ins = fn(self.eng[e])
        self.cnt[e] += 1
        self.nins += 1
        ins.then_inc(self.sem[e], 1)
        self._mark((self.sem[e], self.cnt[e]), reads, writes)
        return ins

    def dma(self, q, out, in_, reads=(), writes=(), **kw):
        lst = self.dpool[q]
        k = lst[self.dnext[q] % len(lst)]
        self.dnext[q] += 1
        evs = self._deps(reads, writes)
        if self.dcnt[k] > 0:
            evs.append((self.dsem[k], 16 * self.dcnt[k]))
        self._wait(q, evs)
        ins = self.eng[q].dma_start(out=out, in_=in_, **kw)
        self.dcnt[k] += 1
        self.nins += 1
        ins.then_inc(self.dsem[k], 16)
        self._mark((self.dsem[k], 16 * self.dcnt[k]), reads, writes)
        return ins

    def finish(self, bufs):
        evs = []
        for b in bufs:
            if b.w is not None:
                evs.append(b.w)
        self._wait("sp", evs)


def _bf(a):
    return np.asarray(a, np.float32).astype(ml_dtypes.bfloat16)


def make_consts(S):
    NT = S // 128
    c = {}
    c["ident_bf"] = _bf(np.eye(128))
    c["ident_f"] = np.eye(128, dtype=np.float32)
    kk = np.arange(128)[:, None]
    qq = np.arange(128)[None, :]
    tab = np.zeros((128, 8, 256), np.float64)
    for h in range(8):
        own = np.where(qq >= kk, -8.0 * SLOPES_SWA[h] * (qq - kk), NEG)
        nxt = np.where(qq < kk, -8.0 * SLOPES_SWA[h] * (128 + qq - kk), NEG)
        tab[:, h, :128] = own
        tab[:, h, 128:] = nxt
    c["swa_tab"] = _bf(tab)
    c["cmask"] = _bf(np.where(qq >= kk, 0.0, NEG))
    o = np.arange(NT + 2)
    al = SLOPES_MOBA[None, :, None] * (np.arange(128)[:, None, None] + 128.0 * (o[None, None, :] - NT))
    c["moba_al"] = al.astype(np.float32)
    jj = np.arange(16)[:, None]
    nn = np.arange(16)[None, :]
    bm = np.where(nn < jj, 0.0, -1e30).astype(np.float32)
    c["blkmask"] = np.broadcast_to(bm[None], (128, 16, 16)).copy()
    E = np.zeros((128, 16 * 128), np.float32)
    for n in range(16):
        E[n, 128 * n:128 * (n + 1)] = 1.0
    c["etab"] = _bf(E)
    T = np.zeros((128, 1920), np.float32)
    for r in range(128):
        b = 7 + 15 * (r // 16)
        T[r, 16 * b + (r % 16)] = 1.0
    c["tsel"] = _bf(T)
    rr = np.arange(128)[:, None] // 16
    cc = np.arange(128)[None, :] // 16
    c["tmask"] = (cc >= rr).astype(np.float32)
    mv = np.array([0, -1, -2, -3, -4, -5, -6, -7, 0, 1, 2, 3, 4, 5, 6, 7, 8], np.float32)
    c["mvals"] = np.broadcast_to(mv[None, None, :], (64, 32, 17)).copy()
    c["cvals"] = np.broadcast_to(np.arange(CG, dtype=np.float32)[None, None, :], (64, 32, CG)).copy()
    return c


CONST_SHAPES = None


def build(S, L, flags=None):
    flags = flags or {}
    NT = S // 128
    NG = S // GT
    consts = make_consts(S)
    nc = bass.Bass("TRN2", target_bir_lowering=False)

    def din(name, shape, dt=F32):
        return nc.dram_tensor(name, list(shape), dt, kind="ExternalInput").ap()

    x_in = din("x", [S, D])
    norm_mix = din("norm_mix", [L, D])
    w_in = din("w_in", [L, D, 5120])
    sinks = din("sinks", [L, 8])
    lam_re = din("lam_re", [L, SG, SP])
    lam_im = din("lam_im", [L, SG, SP])
    log_step = din("log_step", [L, SG])
    b_re = din("b_re", [L, SG, SP, 16])
    b_im = din("b_im", [L, SG, SP, 16])
    c_re = din("c_re", [L, SG, 16, SP])
    c_im = din("c_im", [L, SG, 16, SP])
    d_skip = din("d_skip", [L, SSM_W])
    w_glu = din("w_glu", [L, SSM_W, 2 * SSM_W])
    w_o_swa = din("w_o_swa", [L, 512, D])
    w_o_ssm = din("w_o_ssm", [L, 512, D])
    w_o_moba = din("w_o_moba", [L, 512, D])
    w_out = din("w_out", [L, D, D])
    norm_ffn = din("norm_ffn", [L, D])
    w_ff1 = din("w_ff1", [L, D, DFF])
    w_ff2 = din("w_ff2", [L, DFF, D])
    norm_final = din("norm_final", [1, D])
    cd = {}
    for k, v in consts.items():
        cd[k] = din("c_" + k, v.shape, BF16 if v.dtype == ml_dtypes.bfloat16 else F32)
    out = nc.dram_tensor("out", [S, D], F32, kind="ExternalOutput").ap()
    xs = nc.dram_tensor("xs", [S, D], F32, kind="Internal").ap()
    dbg_t = nc.dram_tensor("dbg", [S, D], F32, kind="ExternalOutput").ap() if flags.get("dbg") else None
    b_dbg = Buf("dbg")

    def dump_x1(c, x1, b_x1, r0, stage):
        if flags.get("dbg") == stage:
            c.dma("sp", dbg_t[r0:r0 + GT, :].rearrange("(t p) d -> p t d", p=128), x1[:], reads=[b_x1], writes=[b_dbg])

    es = ExitStack()
    with es:
        c = Ctx(nc, es)

        def sb(name, shape, dt):
            return es.enter_context(nc.sbuf_tensor(name, list(shape), dt))

        pbank = [es.enter_context(nc.psum_tensor(f"pb{i}", [128, 512], F32)) for i in range(8)]
        pbuf = [Buf(f"pb{i}") for i in range(8)]
        pstate = {"n": 0}

        def psum():
            i = pstate["n"] % 6
            pstate["n"] += 1
            return pbank[i], pbuf[i]

        postate = {"n": 0}

        def psum_acc():
            i = 6 + postate["n"] % 2
            postate["n"] += 1
            return pbank[i], pbuf[i]

        ident_bf = sb("ident_bf", [128, 128], BF16)
        ident_f = sb("ident_f", [128, 128], F32)
        swa_tab = sb("swa_tab", [128, 8, 256], BF16)
        cmask = sb("cmask", [128, 128], BF16)
        moba_al = sb("moba_al", [128, 8, NT + 2], F32)
        blkmask = sb("blkmask", [128, 16, 16], F32)
        etab = sb("etab", [128, 16 * 128], BF16)
        tsel = sb("tsel", [128, 1920], BF16)
        tmask = sb("tmask", [128, 128], F32)
        ones_f = sb("ones_f", [128, 64], F32)
        bconst = Buf("consts")
        for nm, t in [("ident_bf", ident_bf), ("ident_f", ident_f), ("swa_tab", swa_tab), ("cmask", cmask),
                      ("moba_al", moba_al), ("blkmask", blkmask), ("etab", etab), ("tsel", tsel),
                      ("tmask", tmask)]:
            c.dma("sp", t[:], cd[nm], writes=[bconst])
        c.op("dve", lambda e: e.memset(ones_f[:], 1.0), writes=[bconst])

        NWS = 4
        wslot = [sb(f"wslot{i}", [128, 4096], BF16) for i in range(NWS)]
        wbuf = [Buf(f"wslot{i}") for i in range(NWS)]
        wstate = {"n": 0}

        def wload(src_ap, shape):
            i = wstate["n"] % NWS
            wstate["n"] += 1
            n = int(np.prod(shape[1:]))
            assert n <= 4096
            view = wslot[i][0:shape[0], 0:n]
            if len(shape) == 3:
                view = view.rearrange("p (a b) -> p a b", a=shape[1])
            c.dma("pool", view, src_ap, writes=[wbuf[i]], max_dma_last_dim=4096)
            return view, wbuf[i]

        x1 = sb("x1", [128, NTG, D], F32)
        b_x1 = Buf("x1")
        gain_mix = sb("gain_mix", [128, D], F32)
        gain_ffn = sb("gain_ffn", [128, D], F32)
        gain_fin = sb("gain_fin", [128, D], F32)
        b_gmix, b_gffn, b_gfin = Buf("gmix"), Buf("gffn"), Buf("gfin")
        hb = [sb(f"hb{i}", [128, D], BF16) for i in range(2)]
        b_hb = [Buf(f"hb{i}") for i in range(2)]
        hT = sb("hT", [128, KD, GT], BF16)
        b_hT = Buf("hT")
        ss = sb("ss", [128, 2 * NTG], F32)
        rstd = sb("rstd", [128, 2 * NTG], F32)
        b_ss = Buf("ss")
        b_rstd = Buf("rstd")

        HM = sb("HM", [128, 4096], BF16)
        b_HM = Buf("HM")
        hid = HM[:, 0:2048].rearrange("p (k n) -> p k n", k=8)
        mixT = HM[:, 2048:4096].rearrange("p (k n) -> p k n", k=8)
        b_mixT = b_hid = b_HM
        HMf = HM[:].bitcast(F32)
        ZM = sb("ZM", [128, 2048], F32)
        b_ZM = Buf("ZM")
        macc = ZM[:, :].rearrange("p (k n) -> p k n", k=8)
        b_macc = b_ZM
        QQ = sb("QQ", [128, 4096], BF16)
        b_QQ = Buf("QQ")
        QQf = QQ[:].bitcast(F32)
        tmpf = [sb(f"tmpf{i}", [128, 512], F32) for i in range(3)]
        b_tmpf = [Buf(f"tmpf{i}") for i in range(3)]
        tstate = {"n": 0}

        def tmp():
            i = tstate["n"] % 3
            tstate["n"] += 1
            return tmpf[i], b_tmpf[i]

        oTa = sb("oTa", [64, NH, GT], BF16)
        oTm = sb("oTm", [64, NH, GT], BF16)
        zgT = sb("zgT", [128, 4, GT], BF16)
        b_oTa, b_oTm, b_zgT = Buf("oTa"), Buf("oTm"), Buf("zgT")

        use = flags.get("use", "abc") if flags.get("mixers", True) else ""
        QTa = QQ[:, 0:1024].rearrange("p (a n) -> p a n", a=4)
        QTm = QQ[:, 1024:2048].rearrange("p (a n) -> p a n", a=4)
        QTf = QQf[:, 1024:2048].rearrange("p (a n) -> p a n", a=4)
        b_QTa = b_QTm = b_QTf = b_QQ
        KTa = sb("KTa", [128, 128 + GT], BF16)
        b_KTa = Buf("KTa")
        Va = sb("Va", [128, 1 + NTG, 2, 65], BF16)
        b_Va = Buf("Va")
        KTm = sb("KTm", [128, S], BF16)
        b_KTm = Buf("KTm")
        Vm = sb("Vm", [128, NT, 2, 65], BF16)
        b_Vm = Buf("Vm")
        kmean = sb("kmean", [128, 16], F32)
        b_kmean = Buf("kmean")
        esink = sb("esink", [128, 8], F32)
        b_esink = Buf("esink")
        PTs = [sb(f"PT{i}", [128, 512], BF16) for i in range(4)]
        b_PTs = [Buf(f"PT{i}") for i in range(4)]
        ptstate = {"n": 0}

        def ptbuf():
            i = ptstate["n"] % 4
            ptstate["n"] += 1
            return PTs[i], b_PTs[i]

        gm = sb("gm", [128, 2 * NH, 16], F32)
        mx8 = sb("mx8", [128, 2 * NH, 8], F32)
        thr = sb("thr", [128, 2 * NH], F32)
        selb = sb("selb", [128, 2 * NH, 16], F32)
        selb16 = sb("selb16", [128, 2 * NH, 16], BF16)
        selT = sb("selT", [128, NH, GT], BF16)
        b_gm, b_mx8, b_thr, b_selb, b_selT = Buf("gm"), Buf("mx8"), Buf("thr"), Buf("selb"), Buf("selT")
        rden = sb("rden", [128, GT], F32)
        b_rden = Buf("rden")
        c.op("dve", lambda e: e.memset(selT[:], 0.0), writes=[b_selT])
        c.op("dve", lambda e: e.memset(kmean[:], 0.0), writes=[b_kmean])
        c.op("dve", lambda e: e.memset(Va[:], 1.0), writes=[b_Va])
        c.op("dve", lambda e: e.memset(Vm[:], 1.0), writes=[b_Vm])

        def attn_finish(po, bpo, dst, b_dst, h, sink):
            if sink:
                c.op("dve", lambda e: e.tensor_scalar(out=rden[64:65, :], in0=po[64:65, 0:GT],
                                                      scalar1=esink[64:65, h:h + 1], scalar2=None, op0=ALU.add),
                     reads=[bpo, b_esink], writes=[b_rden])
                c.op("dve", lambda e: e.reciprocal(out=rden[64:65, :], in_=rden[64:65, :]),
                     reads=[b_rden], writes=[b_rden])
            else:
                c.op("dve", lambda e: e.reciprocal(out=rden[64:65, :], in_=po[64:65, 0:GT]),
                     reads=[bpo], writes=[b_rden])
            pbc, bpbc = psum()
            c.op("pe", lambda e: e.matmul(pbc[0:64, 0:GT], lhsT=ones_f[64:65, 0:64], rhs=rden[64:65, :],
                                          start=True, stop=True),
                 reads=[b_rden, bconst], writes=[bpbc])
            tt, btt = tmp()
            c.op("act", lambda e: e.copy(out=tt[0:64, 0:GT], in_=po[0:64, 0:GT]), reads=[bpo, b_rden], writes=[btt])
            c.op("dve", lambda e: e.tensor_tensor(out=dst[0:64, h, :], in0=tt[0:64, 0:GT], in1=pbc[0:64, 0:GT],
                                                  op=ALU.mult),
                 reads=[btt, bpbc], writes=[b_dst])


        WinT = sb("WinT", [128, SG, 2, 64], BF16)
        Wout = sb("Wout", [64, 2, SG, 128], BF16)
        Toep = sb("Toep", [128, SG, 128], BF16)
        b_WinT, b_Wout, b_Toep = Buf("WinT"), Buf("Wout"), Buf("Toep")
        A1 = sb("A1", [64, 2, SG], F32)
        A2 = sb("A2", [64, 2, SG], F32)
        b_A = Buf("A12")
        cosL = sb("cosL", [64, SG, CG], F32)
        sinL = sb("sinL", [64, SG, CG], F32)
        rtab = sb("rtab", [64, SG, CG], F32)
        b_tabL = Buf("tabL")
        Zb = ZM[0:64, :].rearrange("p (r g c) -> p r g c", r=2, g=SG)
        Zr = QQf[0:64, :].rearrange("p (r g c) -> p r g c", r=2, g=SG)
        Tt = HMf[0:64, :].rearrange("p (r g c) -> p r g c", r=2, g=SG)
        Rb = sb("Rb", [64, 2, SG, CG], BF16)
        b_Zb, b_Zr, b_Tt, b_Rb = b_ZM, b_QQ, b_HM, Buf("Rb")
        carry = sb("carry", [64, 2, SG], F32)
        b_carry = Buf("carry")
        uT = sb("uT", [128, 4, GT], BF16)
        Ug = sb("Ug", [128, SG, CG], BF16)
        yg = sb("yg", [128, SG, CG], BF16)
        ygT = sb("ygT", [128, 4, GT], BF16)
        b_uT, b_Ug, b_yg, b_ygT = Buf("uT"), Buf("Ug"), Buf("yg"), Buf("ygT")
        s_lr = sb("s_lr", [64, SG], F32)
        s_li = sb("s_li", [64, SG], F32)
        s_dt = sb("s_dt", [64, SG], F32)
        s_a = [sb(f"s_a{i}", [64, SG], F32) for i in range(6)]
        wsf = [wslot[i][:].bitcast(F32) for i in range(NWS)]
        v3 = lambda ap, a, b: ap.rearrange("p (a b) -> p a b", a=a)
        s_m = [v3(wsf[0][0:64, 0:1024], SG, CG), v3(wsf[0][0:64, 1024:2048], SG, CG), v3(wsf[1][0:64, 0:1024], SG, CG)]
        s_mi = v3(wslot[1][:].bitcast(I32)[0:64, 1024:2048], SG, CG)
        s_w = [wsf[2][0:64, 0:1024].rearrange("p (g s h) -> p g s h", g=8, s=8),
               wsf[2][0:64, 1024:2048].rearrange("p (g s h) -> p g s h", g=8, s=8),
               wsf[3][0:64, 0:1024].rearrange("p (g s h) -> p g s h", g=8, s=8)]
        s_winb = wslot[3][0:64, 2048:4096].rearrange("p (r g n) -> p r g n", r=2, g=8)
        s_win = ZM[0:64, :].rearrange("p (r g n) -> p r g n", r=2, g=8)
        s_br = v3(HMf[0:64, 0:512], SG, 16)
        s_bi = v3(HMf[0:64, 512:1024], SG, 16)
        s_bbr = v3(HMf[0:64, 1024:1536], SG, 16)
        s_bbi = v3(HMf[0:64, 1536:2048], SG, 16)
        s_cr = v3(QQf[0:64, 0:512], SG, 16)
        s_ci = v3(QQf[0:64, 512:1024], SG, 16)
        s_E = [v3(QQf[0:64, 1024:1568], SG, 17), sb("s_Ei", [64, SG, 17], F32)]
        mv_t = v3(wsf[2][0:64, 0:544], SG, 17)
        cv_t = v3(wsf[2][0:64, 0:1024], SG, CG)
        s_dcol = sb("s_dcol", [128, SG], F32)
        s_tm = sb("s_tm", [128, 128], F32)
        bS = Buf("ssm_setup")
        SW = [bS, b_ZM, b_HM, b_QQ] + wbuf
        TWO_PI = 2.0 * math.pi

        def sincos(dst_cos, dst_sin, ang_y, n3):
            tmpy = s_m[2][:, :, 0:n3]
            for dst, off in ((dst_sin, 0.0), (dst_cos, 0.25)):
                c.op("dve", lambda e: e.tensor_scalar(out=tmpy, in0=ang_y, scalar1=off, scalar2=None, op0=ALU.add),
                     reads=SW, writes=SW)
                c.op("dve", lambda e: e.tensor_copy(out=s_mi[:, :, 0:n3], in_=tmpy), reads=SW, writes=SW)
                c.op("dve", lambda e: e.tensor_copy(out=dst, in_=s_mi[:, :, 0:n3]), reads=SW, writes=SW)
                c.op("dve", lambda e: e.tensor_tensor(out=tmpy, in0=tmpy, in1=dst, op=ALU.subtract),
                     reads=SW, writes=SW)
                c.op("dve", lambda e: e.tensor_scalar(out=dst, in0=tmpy, scalar1=0.5, scalar2=None, op0=ALU.is_ge),
                     reads=SW, writes=SW)
                c.op("dve", lambda e: e.tensor_tensor(out=tmpy, in0=tmpy, in1=dst, op=ALU.subtract),
                     reads=SW, writes=SW)
                c.op("act", lambda e: e.activation(out=dst, in_=tmpy, func=AF.Sin, scale=TWO_PI),
                     reads=SW, writes=SW)

        def ssm_setup(l):
            d2 = lambda e, o, a, b, op: e.tensor_tensor(out=o, in0=a, in1=b, op=op)
            with nc.allow_non_contiguous_dma(reason="small ssm parameter transposes"):
                c.dma("sp", s_lr[:], lam_re[l].rearrange("g p -> p g"), writes=SW)
                c.dma("sp", s_li[:], lam_im[l].rearrange("g p -> p g"), writes=SW)
                c.dma("sp", s_cr[:], c_re[l].rearrange("g h p -> p g h"), writes=SW)
                c.dma("sp", s_ci[:], c_im[l].rearrange("g h p -> p g h"), writes=SW)
                for sg_ in range(8):
                    c.dma("sp", s_dcol[16 * sg_:16 * (sg_ + 1), :], d_skip[l].rearrange("(g h) -> h g", h=16),
                          writes=SW)
            c.dma("sp", s_dt[:], log_step[l:l + 1, :].broadcast_to([64, SG]), writes=SW)
            c.dma("sp", s_br[:], b_re[l].rearrange("g p h -> p g h"), writes=SW)
            c.dma("sp", s_bi[:], b_im[l].rearrange("g p h -> p g h"), writes=SW)
            c.op("act", lambda e: e.activation(out=s_dt[:], in_=s_dt[:], func=AF.Exp), reads=SW, writes=SW)
            ldr, ldi = s_a[0], s_a[1]
            c.op("dve", lambda e: d2(e, ldr[:], s_lr[:], s_dt[:], ALU.mult), reads=SW, writes=SW)
            c.op("dve", lambda e: d2(e, ldi[:], s_li[:], s_dt[:], ALU.mult), reads=SW, writes=SW)
            Er, Ei = s_E
            c.dma("sp", mv_t, cd["mvals"], reads=SW, writes=SW)
            mag = s_m[0][:, :, 0:17]
            yy = s_m[1][:, :, 0:17]
            c.op("dve", lambda e: d2(e, mag, mv_t, ldr[:].unsqueeze(2).broadcast_to([64, SG, 17]), ALU.mult),
                 reads=SW + [bconst], writes=SW)
            c.op("act", lambda e: e.activation(out=mag, in_=mag, func=AF.Exp), reads=SW, writes=SW)
            c.op("dve", lambda e: d2(e, yy, mv_t, ldi[:].unsqueeze(2).broadcast_to([64, SG, 17]), ALU.mult),
                 reads=SW + [bconst], writes=SW)
            c.op("dve", lambda e: e.tensor_scalar(out=yy, in0=yy, scalar1=1.0 / TWO_PI, scalar2=32.0, op0=ALU.mult,
                                                  op1=ALU.add), reads=SW, writes=SW)
            sincos(Er[:], Ei[:], yy, 17)
            c.op("dve", lambda e: d2(e, Er[:], Er[:], mag, ALU.mult), reads=SW, writes=SW)
            c.op("dve", lambda e: d2(e, Ei[:], Ei[:], mag, ALU.mult), reads=SW, writes=SW)
            nr, den, t0, t1, fre, fim = s_a[2], s_a[3], s_a[4], s_a[5], s_a[0], s_a[1]
            ni = Ei[:, :, 9]
            c.op("dve", lambda e: e.tensor_scalar(out=nr[:], in0=Er[:, :, 9], scalar1=-1.0, scalar2=None, op0=ALU.add),
                 reads=SW, writes=SW)
            c.op("dve", lambda e: d2(e, den[:], s_lr[:], s_lr[:], ALU.mult), reads=SW, writes=SW)
            c.op("dve", lambda e: d2(e, t0[:], s_li[:], s_li[:], ALU.mult), reads=SW, writes=SW)
            c.op("dve", lambda e: d2(e, den[:], den[:], t0[:], ALU.add), reads=SW, writes=SW)
            c.op("dve", lambda e: e.reciprocal(out=den[:], in_=den[:]), reads=SW, writes=SW)
            c.op("dve", lambda e: d2(e, t0[:], nr[:], s_lr[:], ALU.mult), reads=SW, writes=SW)
            c.op("dve", lambda e: d2(e, t1[:], ni, s_li[:], ALU.mult), reads=SW, writes=SW)
            c.op("dve", lambda e: d2(e, t0[:], t0[:], t1[:], ALU.add), reads=SW, writes=SW)
            c.op("dve", lambda e: d2(e, fre[:], t0[:], den[:], ALU.mult), reads=SW, writes=SW)
            c.op("dve", lambda e: d2(e, t0[:], ni, s_lr[:], ALU.mult), reads=SW, writes=SW)
            c.op("dve", lambda e: d2(e, t1[:], nr[:], s_li[:], ALU.mult), reads=SW, writes=SW)
            c.op("dve", lambda e: d2(e, t0[:], t0[:], t1[:], ALU.subtract), reads=SW, writes=SW)
            c.op("dve", lambda e: d2(e, fim[:], t0[:], den[:], ALU.mult), reads=SW, writes=SW)
            fr_b = fre[:].unsqueeze(2).broadcast_to([64, SG, 16])
            fi_b = fim[:].unsqueeze(2).broadcast_to([64, SG, 16])
            w0 = s_m[0][:, :, 0:16]
            w1 = s_m[1][:, :, 0:16]
            c.op("dve", lambda e: d2(e, w0, s_br[:], fr_b, ALU.mult), reads=SW, writes=SW)
            c.op("dve", lambda e: d2(e, w1, s_bi[:], fi_b, ALU.mult), reads=SW, writes=SW)
            c.op("dve", lambda e: d2(e, s_bbr[:], w0, w1, ALU.subtract), reads=SW, writes=SW)
            c.op("dve", lambda e: d2(e, w0, s_bi[:], fr_b, ALU.mult), reads=SW, writes=SW)
            c.op("dve", lambda e: d2(e, w1, s_br[:], fi_b, ALU.mult), reads=SW, writes=SW)
            c.op("dve", lambda e: d2(e, s_bbi[:], w0, w1, ALU.add), reads=SW, writes=SW)
            c.op("dve", lambda e: e.tensor_copy(out=A1[:, 0, :], in_=Er[:, :, 16]), reads=SW, writes=[b_A])
            c.op("dve", lambda e: e.tensor_copy(out=A1[:, 1, :], in_=Er[:, :, 16]), reads=SW, writes=[b_A])
            c.op("dve", lambda e: e.tensor_copy(out=A2[:, 1, :], in_=Ei[:, :, 16]), reads=SW, writes=[b_A])
            c.op("dve", lambda e: e.tensor_scalar(out=A2[:, 0, :], in0=Ei[:, :, 16], scalar1=-1.0, scalar2=None,
                                                  op0=ALU.mult), reads=SW, writes=[b_A])
            ldi8 = s_a[2]
            c.op("dve", lambda e: d2(e, ldi8[:], s_li[:], s_dt[:], ALU.mult), reads=SW, writes=SW)
            yy2 = s_m[1][:]
            c.dma("sp", cv_t, cd["cvals"], reads=SW, writes=SW)
            c.op("dve", lambda e: d2(e, yy2, cv_t, ldi8[:].unsqueeze(2).broadcast_to([64, SG, CG]), ALU.mult),
                 reads=SW + [bconst], writes=SW)
            c.op("dve", lambda e: e.tensor_scalar(out=yy2, in0=yy2, scalar1=float(SK) / TWO_PI, scalar2=8.0,
                                                  op0=ALU.mult, op1=ALU.add), reads=SW, writes=SW)
            sincos(cosL[:], sinL[:], yy2, CG)
            c.op("dve", lambda e: e.tensor_copy(out=cosL[:], in_=cosL[:]), reads=SW, writes=[b_tabL])
            c.op("dve", lambda e: d2(e, ldi8[:], s_lr[:], s_dt[:], ALU.mult), reads=SW, writes=SW)
            c.op("act", lambda e: e.activation(out=ldi8[:], in_=ldi8[:], func=AF.Exp, scale=float(SK)),
                 reads=SW, writes=SW)
            c.op("dve", lambda e: e.tensor_copy(out=rtab[:], in_=ldi8[:].unsqueeze(2).broadcast_to([64, SG, CG])),
                 reads=SW, writes=[b_tabL])
            c.op("dve", lambda e: e.memset(rtab[:, :, 0:1], 0.0), reads=SW, writes=[b_tabL])
            for gb in range(4):
                g0 = 8 * gb
                gs = slice(g0, g0 + 8)

                def bc_m(t, i0):
                    return t[:, gs, i0:i0 + 8].unsqueeze(3).broadcast_to([64, 8, 8, 16])

                def bc_h(t):
                    return t[:, gs, :].unsqueeze(2).broadcast_to([64, 8, 8, 16])

                wre = s_win[:, 0, :, :].rearrange("p g (s h) -> p g s h", h=16)
                wim = s_win[:, 1, :, :].rearrange("p g (s h) -> p g s h", h=16)
                c.op("dve", lambda e: d2(e, s_w[0][:], bc_m(Er, 0), bc_h(s_bbr), ALU.mult), reads=SW, writes=SW)
                c.op("dve", lambda e: d2(e, s_w[1][:], bc_m(Ei, 0), bc_h(s_bbi), ALU.mult), reads=SW, writes=SW)
                c.op("dve", lambda e: d2(e, wre, s_w[0][:], s_w[1][:], ALU.subtract), reads=SW, writes=SW)
                c.op("dve", lambda e: d2(e, s_w[0][:], bc_m(Er, 0), bc_h(s_bbi), ALU.mult), reads=SW, writes=SW)
                c.op("dve", lambda e: d2(e, s_w[1][:], bc_m(Ei, 0), bc_h(s_bbr), ALU.mult), reads=SW, writes=SW)
                c.op("dve", lambda e: d2(e, wim, s_w[0][:], s_w[1][:], ALU.add), reads=SW, writes=SW)
                c.op("act", lambda e: e.copy(out=s_winb[:], in_=s_win[:]), reads=SW, writes=SW)
                for gi in range(8):
                    pb, bpb = psum()
                    for ri in range(2):
                        c.op("pe", lambda e: e.transpose(out=pb[:, 64 * ri:64 * (ri + 1)], in_=s_win[:, ri, gi, :],
                                                         identity=ident_f[0:64, 0:64]),
                             reads=SW + [bconst], writes=[bpb])
                    c.op("act", lambda e: e.copy(out=WinT[:, g0 + gi, :, :],
                                                 in_=pb[:, 0:128].rearrange("p (r q) -> p r q", r=2)),
                         reads=[bpb], writes=[b_WinT])
                wo_re = Wout[:, 0, gs, :].rearrange("p g (s h) -> p g s h", h=16)
                wo_im = Wout[:, 1, gs, :].rearrange("p g (s h) -> p g s h", h=16)
                c.op("dve", lambda e: d2(e, s_w[0][:], bc_m(Er, 8), bc_h(s_cr), ALU.mult), reads=SW, writes=SW)
                c.op("dve", lambda e: d2(e, s_w[1][:], bc_m(Ei, 8), bc_h(s_ci), ALU.mult), reads=SW, writes=SW)
                c.op("dve", lambda e: d2(e, wo_re, s_w[0][:], s_w[1][:], ALU.subtract), reads=SW, writes=[b_Wout])
                c.op("dve", lambda e: d2(e, s_w[0][:], bc_m(Ei, 8), bc_h(s_cr), ALU.mult), reads=SW, writes=SW)
                c.op("dve", lambda e: d2(e, s_w[1][:], bc_m(Er, 8), bc_h(s_ci), ALU.mult), reads=SW, writes=SW)
                c.op("dve", lambda e: d2(e, s_w[2][:], s_w[0][:], s_w[1][:], ALU.add), reads=SW, writes=SW)
                c.op("dve", lambda e: e.tensor_scalar(out=wo_im, in0=s_w[2][:], scalar1=-1.0, scalar2=None, op0=ALU.mult),
                     reads=SW, writes=[b_Wout])
                for gi in range(8):
                    g = g0 + gi
                    pb, bpb = psum()
                    for ri in range(2):
                        c.op("pe", lambda e: e.matmul(pb[:, 0:128], lhsT=s_winb[:, ri, gi, :], rhs=Wout[:, ri, g, :],
                                                      start=(ri == 0), stop=(ri == 1)),
                             reads=SW + [b_Wout], writes=[bpb])
                    c.op("dve", lambda e: d2(e, s_tm[:], pb[:, 0:128], tmask[:], ALU.mult),
                         reads=[bpb, bconst, bS], writes=SW)
                    c.op("dve", lambda e: e.scalar_tensor_tensor(out=Toep[:, g, :], in0=ident_f[:],
                                                                 scalar=s_dcol[:, g:g + 1], in1=s_tm[:],
                                                                 op0=ALU.mult, op1=ALU.add),
                         reads=SW + [bconst], writes=[b_Toep])
            c.op("dve", lambda e: e.memset(carry[:], 0.0), writes=[b_carry])

        epsc = sb("epsc", [128, 1], F32)
        c.op("dve", lambda e: e.memset(epsc[:], EPS), writes=[bconst])

        def rmsnorm_to_T(gain, b_gain, dstT, b_dstT, sscol):
            for t in range(NTG):
                c.op("act", lambda e: e.activation(out=hb[t % 2][:], in_=x1[:, t, :], func=AF.Square,
                                                   accum_out=ss[:, sscol + t:sscol + t + 1]),
                     reads=[b_x1], writes=[b_hb[t % 2], b_ss])
            c.op("act", lambda e: e.activation(out=rstd[:, sscol:sscol + NTG], in_=ss[:, sscol:sscol + NTG],
                                               func=AF.Sqrt, scale=1.0 / D, bias=epsc[:]),
                 reads=[b_ss, bconst], writes=[b_rstd])
            c.op("dve", lambda e: e.reciprocal(out=rstd[:, sscol:sscol + NTG], in_=rstd[:, sscol:sscol + NTG]),
                 reads=[b_rstd], writes=[b_rstd])
            for t in range(NTG):
                h_ = hb[t % 2]
                bh = b_hb[t % 2]
                c.op("dve", lambda e: e.scalar_tensor_tensor(out=h_[:], in0=x1[:, t, :],
                                                             scalar=rstd[:, sscol + t:sscol + t + 1],
                                                             in1=gain[:], op0=ALU.mult, op1=ALU.mult),
                     reads=[b_x1, b_rstd, b_gain], writes=[bh])
                pb, bpb = psum()
                pv = pb[:].bitcast(BF16).rearrange("p (k n) -> p k n", k=8)
                for k in range(KD):
                    c.op("pe", lambda e: e.transpose(out=pv[:, k, :], in_=h_[:, 128 * k:128 * (k + 1)],
                                                     identity=ident_bf[:]),
                         reads=[bh, bconst], writes=[bpb])
                c.op("act", lambda e: e.copy(out=dstT[:, :, 128 * t:128 * (t + 1)], in_=pv),
                     reads=[bpb], writes=[b_dstT])

        def load_gain(dst, bdst, src_row):
            c.dma("sp", dst[:], src_row.broadcast_to([128, D]), writes=[bdst])

        def proj_fm(dst_fn, lhs_view, bw, col0, rhsT, b_rhs, nk, ncols=128):
            pb, bpb = psum()
            for k in range(nk):
                c.op("pe", lambda e: e.matmul(pb[0:ncols, 0:GT], lhsT=lhs_view[:, k, col0:col0 + ncols],
                                              rhs=rhsT[:, k, :], start=(k == 0), stop=(k == nk - 1)),
                     reads=[bw, b_rhs], writes=[bpb])
            dst_fn(pb, bpb)

        load_gain(gain_fin, b_gfin, norm_final[0:1, :])
        for l in range(L):
            if l > 0:
                c.new_epoch(es, f"L{l}")
            load_gain(gain_mix, b_gmix, norm_mix[l:l + 1, :])
            load_gain(gain_ffn, b_gffn, norm_ffn[l:l + 1, :])
            xsrc = x_in if l == 0 else xs
            c.dma("sp", esink[:], sinks[l:l + 1, :].broadcast_to([128, 8]), writes=[b_esink])
            c.op("act", lambda e: e.activation(out=esink[:], in_=esink[:], func=AF.Exp),
                 reads=[b_esink], writes=[b_esink])
            win_v = w_in[l].rearrange("(k p) n -> p k n", p=128)
            wout_v = w_out[l].rearrange("(k p) n -> p k n", p=128)
            wff1_v = w_ff1[l].rearrange("(k p) n -> p k n", p=128)
            wff2_v = w_ff2[l].rearrange("(k p) n -> p k n", p=128)
            wosw_v = w_o_swa[l].rearrange("(h d) n -> d h n", d=64)
            womb_v = w_o_moba[l].rearrange("(h d) n -> d h n", d=64)
            wossm_v = w_o_ssm[l].rearrange("(k p) n -> p k n", p=128)
            b_xg = [Buf(f"xg{g}") for g in range(NG)]
            if "b" in use:
                ssm_setup(l)
            wglu_v = w_glu[l].rearrange("(k p) n -> p k n", p=128)
            for G in range(NG):
                r0 = G * GT
                c.dma("sp", x1[:], xsrc[r0:r0 + GT, :].rearrange("(t p) d -> p t d", p=128),
                      reads=[b_xg[G]], writes=[b_x1])
                dump_x1(c, x1, b_x1, r0, "load")
                rmsnorm_to_T(gain_mix, b_gmix, hT, b_hT, 0)

                if "a" not in use:
                    c.op("dve", lambda e: e.memset(oTa[:], 0.0), writes=[b_oTa])
                if "c" not in use:
                    c.op("dve", lambda e: e.memset(oTm[:], 0.0), writes=[b_oTm])
                if "b" not in use:
                    c.op("dve", lambda e: e.memset(zgT[:], 0.0), writes=[b_zgT])

                def qkv_proj(qc0, kc0, QT, b_QT, KTw, b_KTw, kcol0, Vw, b_Vw, vslot0, want_f32):
                    kw, bkw = wload(win_v[:, :, kc0:kc0 + 256], [128, 8, 256])
                    pb, bpb = psum()
                    for k in range(KD):
                        c.op("pe", lambda e: e.matmul(pb[:, 0:GT], lhsT=kw[:, k, 0:128], rhs=hT[:, k, :],
                                                      start=(k == 0), stop=(k == KD - 1)),
                             reads=[bkw, b_hT], writes=[bpb])
                    c.op("act", lambda e: e.copy(out=KTw[:, kcol0:kcol0 + GT], in_=pb[:, 0:GT]),
                         reads=[bpb], writes=[b_KTw])
                    if want_f32:
                        c.op("dve", lambda e: e.tensor_reduce(out=kmean[:, G:G + 1], in_=pb[:, 0:GT],
                                                              axis=AX.X, op=ALU.add),
                             reads=[bpb, b_KTw], writes=[b_kmean])
                        c.op("dve", lambda e: e.tensor_scalar(out=kmean[:, G:G + 1], in0=kmean[:, G:G + 1],
                                                              scalar1=1.0 / GT, scalar2=None, op0=ALU.mult),
                             reads=[b_kmean], writes=[b_kmean])
                    for t in range(NTG):
                        pb, bpb = psum()
                        for k in range(KD):
                            c.op("pe", lambda e: e.matmul(pb[:, 0:128], lhsT=hT[:, k, 128 * t:128 * (t + 1)],
                                                          rhs=kw[:, k, 128:256], start=(k == 0), stop=(k == KD - 1)),
                                 reads=[bkw, b_hT], writes=[bpb])
                        c.op("act", lambda e: e.copy(out=Vw[:, vslot0 + t, :, 0:64],
                                                     in_=pb[:, 0:128].rearrange("p (a b) -> p a b", a=2)),
                             reads=[bpb], writes=[b_Vw])
                    qw, bqw = wload(win_v[:, :, qc0:qc0 + 512], [128, 8, 512])
                    for p in range(4):
                        pb, bpb = psum()
                        for half in range(2):
                            c0_ = 64 * (p + 4 * half)
                            for k in range(KD):
                                c.op("pe", lambda e: e.matmul(pb[64 * half:64 * half + 64, 0:GT],
                                                              lhsT=qw[:, k, c0_:c0_ + 64], rhs=hT[:, k, :],
                                                              start=(k == 0), stop=(k == KD - 1)),
                                     reads=[bqw, b_hT], writes=[bpb])
                        c.op("act", lambda e: e.copy(out=QT[:, p, :], in_=pb[:, 0:GT]),
                             reads=[bpb], writes=[b_QT])
                        if want_f32:
                            c.op("dve", lambda e: e.tensor_copy(out=QTf[:, p, :], in_=pb[:, 0:GT]),
                                 reads=[bpb], writes=[b_QTf])

                if "a" in use:
                    qkv_proj(0, 512, QTa, b_QTa, KTa, b_KTa, 128, Va, b_Va, 1, False)
                    for h in range(NH):
                        p, s_, kv = h % 4, h // 4, h // 4
                        lo = 64 * s_
                        ps_, bps = psum()
                        segs = []
                        if G > 0:
                            segs.append((0, 128, 0, 0, 128, 128))
                        segs.append((128, 256, 128, 0, 256, 0))
                        segs.append((384, 128, 256, 128, 128, 0))
                        for (pc, w, kc, qc, qw_, tc) in segs:
                            c.op("pe", lambda e: e.matmul(ps_[:, pc:pc + w], lhsT=KTa[lo:lo + 64, kc:kc + 128],
                                                          rhs=QTa[lo:lo + 64, p, qc:qc + qw_], start=True, stop=False),
                                 reads=[b_KTa, b_QTa], writes=[bps])
                            c.op("pe", lambda e: e.matmul(ps_[:, pc:pc + w], lhsT=ident_bf[:],
                                                          rhs=swa_tab[:, h, tc:tc + w], start=False, stop=True),
                                 reads=[bconst], writes=[bps])
                        c0 = 0 if G > 0 else 128
                        pt, bpt = ptbuf()
                        c.op("act", lambda e: e.activation(out=pt[:, c0:512], in_=ps_[:, c0:512], func=AF.Exp,
                                                           scale=0.125),
                             reads=[bps], writes=[bpt])
                        po, bpo = psum_acc()
                        pvs = []
                        if G > 0:
                            pvs.append((0, 0, 0))
                        pvs += [(1, 128, 0), (1, 256, 128), (2, 384, 128)]
                        for i, (vs, pc, oc) in enumerate(pvs):
                            c.op("pe", lambda e: e.matmul(po[0:65, oc:oc + 128], lhsT=Va[:, vs, kv, :],
                                                          rhs=pt[:, pc:pc + 128], start=(i == 0),
                                                          stop=(i == len(pvs) - 1)),
                                 reads=[b_Va, bpt], writes=[bpo])
                        attn_finish(po, bpo, oTa, b_oTa, h, True)
                    c.op("act", lambda e: e.copy(out=KTa[:, 0:128], in_=KTa[:, GT:GT + 128]),
                         reads=[b_KTa], writes=[b_KTa])
                    c.op("dve", lambda e: e.tensor_copy(out=Va[:, 0, :, :], in_=Va[:, NTG, :, :]),
                         reads=[b_Va], writes=[b_Va])

                if "c" in use:
                    qkv_proj(1280, 1792, QTm, b_QTm, KTm, b_KTm, G * GT, Vm, b_Vm, G * NTG, True)
                    if G > 0 and not flags.get("nogate"):
                        gm4 = gm[:].rearrange("p (t h) n -> p t h n", t=NTG)
                        for s_ in range(2):
                            lo = 64 * s_
                            pgt, bpgt = psum()
                            for t in range(NTG):
                                for p in range(4):
                                    col = (t * 4 + p) * 16
                                    c.op("pe", lambda e: e.matmul(pgt[:, col:col + 16],
                                                                  lhsT=QTf[lo:lo + 64, p, 128 * t:128 * (t + 1)],
                                                                  rhs=kmean[lo:lo + 64, 0:16], start=True, stop=True),
                                         reads=[b_QTf, b_kmean], writes=[bpgt])
                            c.op("dve", lambda e: e.tensor_tensor(
                                out=gm4[:, :, 4 * s_:4 * s_ + 4, :],
                                in0=pgt[:, 0:128].rearrange("p (t h n) -> p t h n", t=NTG, h=4),
                                in1=blkmask[:, G:G + 1, :].unsqueeze(1).broadcast_to([128, NTG, 4, 16]), op=ALU.add),
                                 reads=[bpgt, bconst], writes=[b_gm])
                        for th in range(2 * NH):
                            c.op("dve", lambda e: e.max(out=mx8[:, th, :], in_=gm[:, th, :]),
                                 reads=[b_gm], writes=[b_mx8])
                        c.op("dve", lambda e: e.tensor_scalar(out=thr[:], in0=mx8[:, :, TOPK - 1], scalar1=-1e29,
                                                              scalar2=None, op0=ALU.max),
                             reads=[b_mx8], writes=[b_thr])
                        for th in range(2 * NH):
                            c.op("dve", lambda e: e.tensor_scalar(out=selb[:, th, :], in0=gm[:, th, :],
                                                                  scalar1=thr[:, th:th + 1], scalar2=-NEG,
                                                                  op0=ALU.is_ge, op1=ALU.mult),
                                 reads=[b_gm, b_thr], writes=[b_selb])
                        c.op("dve", lambda e: e.tensor_scalar(out=selb16[:], in0=selb[:], scalar1=NEG, scalar2=None,
                                                              op0=ALU.add),
                             reads=[b_selb], writes=[b_selb])
                        for hp in range(NH // 2):
                            pst, bpst = psum()
                            pst16 = pst[:].bitcast(BF16)
                            for hh in range(2):
                                h = 2 * hp + hh
                                for t in range(NTG):
                                    c.op("pe", lambda e: e.transpose(
                                        out=pst16[0:16, hh * GT + 128 * t:hh * GT + 128 * (t + 1)],
                                        in_=selb16[:, t * NH + h, :], identity=ident_bf[:]),
                                         reads=[b_selb, bconst], writes=[bpst])
                            c.op("act", lambda e: e.copy(out=selT[0:16, 2 * hp:2 * hp + 2, :],
                                                         in_=pst16[0:16, 0:2 * GT].rearrange("p (a n) -> p a n", a=2)),
                                 reads=[bpst], writes=[b_selT])
                    for h in range(NH):
                        p, s_, kv = h % 4, h // 4, h // 4
                        lo = 64 * s_
                        po, bpo = psum_acc()
                        nmm = 2 * G + 2
                        imm = 0
                        for n in range(G):
                            for kt in (2 * n, 2 * n + 1):
                                ps_, bps = psum()
                                c.op("pe", lambda e: e.matmul(ps_[:, 0:GT], lhsT=KTm[lo:lo + 64, 128 * kt:128 * (kt + 1)],
                                                              rhs=QTm[lo:lo + 64, p, :], start=True, stop=False),
                                     reads=[b_KTm, b_QTm], writes=[bps])
                                c.op("pe", lambda e: e.matmul(ps_[:, 0:GT], lhsT=etab[:, 128 * n:128 * (n + 1)],
                                                              rhs=selT[:, h, :], start=False, stop=True),
                                     reads=[b_selT, bconst], writes=[bps])
                                pt, bpt = ptbuf()
                                o = kt - 2 * G + NT
                                c.op("act", lambda e: e.activation(out=pt[:, 0:GT], in_=ps_[:, 0:GT], func=AF.Exp,
                                                                   scale=0.125, bias=moba_al[:, h, o:o + 1]),
                                     reads=[bps, bconst], writes=[bpt])
                                c.op("pe", lambda e: e.matmul(po[0:65, 0:GT], lhsT=Vm[:, kt, kv, :], rhs=pt[:, 0:GT],
                                                              start=(imm == 0), stop=False),
                                     reads=[b_Vm, bpt], writes=[bpo])
                                imm += 1
                        kt = 2 * G
                        ps_, bps = psum()
                        c.op("pe", lambda e: e.matmul(ps_[:, 0:GT], lhsT=KTm[lo:lo + 64, 128 * kt:128 * (kt + 1)],
                                                      rhs=QTm[lo:lo + 64, p, :], start=True, stop=False),
                             reads=[b_KTm, b_QTm], writes=[bps])
                        c.op("pe", lambda e: e.matmul(ps_[:, 0:128], lhsT=ident_bf[:], rhs=cmask[:], start=False, stop=True),
                             reads=[bconst], writes=[bps])
                        pt, bpt = ptbuf()
                        c.op("act", lambda e: e.activation(out=pt[:, 0:GT], in_=ps_[:, 0:GT], func=AF.Exp, scale=0.125,
                                                           bias=moba_al[:, h, NT:NT + 1]),
                             reads=[bps, bconst], writes=[bpt])
                        c.op("pe", lambda e: e.matmul(po[0:65, 0:GT], lhsT=Vm[:, kt, kv, :], rhs=pt[:, 0:GT],
                                                      start=(imm == 0), stop=False),
                             reads=[b_Vm, bpt], writes=[bpo])
                        kt = 2 * G + 1
                        ps_, bps = psum()
                        c.op("pe", lambda e: e.matmul(ps_[:, 0:128], lhsT=KTm[lo:lo + 64, 128 * kt:128 * (kt + 1)],
                                                      rhs=QTm[lo:lo + 64, p, 128:256], start=True, stop=False),
                             reads=[b_KTm, b_QTm], writes=[bps])
                        c.op("pe", lambda e: e.matmul(ps_[:, 0:128], lhsT=ident_bf[:], rhs=cmask[:], start=False, stop=True),
                             reads=[bconst], writes=[bps])
                        pt, bpt = ptbuf()
                        c.op("act", lambda e: e.activation(out=pt[:, 0:128], in_=ps_[:, 0:128], func=AF.Exp, scale=0.125,
                                                           bias=moba_al[:, h, NT + 1:NT + 2]),
                             reads=[bps, bconst], writes=[bpt])
                        c.op("pe", lambda e: e.matmul(po[0:65, 128:256], lhsT=Vm[:, kt, kv, :], rhs=pt[:, 0:128],
                                                      start=False, stop=True),
                             reads=[b_Vm, bpt], writes=[bpo])
                        attn_finish(po, bpo, oTm, b_oTm, h, False)

                if "b" in use:
                    d2 = lambda e, o, a, b, op: e.tensor_tensor(out=o, in0=a, in1=b, op=op)
                    for uh in range(2):
                        uw, buw = wload(win_v[:, :, 768 + 256 * uh:768 + 256 * (uh + 1)], [128, 8, 256])
                        for j in range(2):
                            pb, bpb = psum()
                            for k in range(KD):
                                c.op("pe", lambda e: e.matmul(pb[:, 0:GT], lhsT=uw[:, k, 128 * j:128 * (j + 1)],
                                                              rhs=hT[:, k, :], start=(k == 0), stop=(k == KD - 1)),
                                     reads=[buw, b_hT], writes=[bpb])
                            c.op("act", lambda e: e.copy(out=uT[:, 2 * uh + j, :], in_=pb[:, 0:GT]),
                                 reads=[bpb], writes=[b_uT])
                    for gh in range(2):
                        pb, bpb = psum()
                        for gi in range(16):
                            g = 16 * gh + gi
                            ct, j = g // 8, g % 8
                            uv = uT[:, ct, :].rearrange("p (c s) -> p s c", s=SK)
                            for sg_ in range(SK):
                                o0 = 16 * (7 + 15 * j - sg_)
                                c.op("pe", lambda e: e.matmul(pb[:, CG * gi:CG * (gi + 1)], lhsT=tsel[:, o0:o0 + 128],
                                                              rhs=uv[:, sg_, :], start=(sg_ == 0), stop=(sg_ == SK - 1)),
                                     reads=[b_uT, bconst], writes=[bpb])
                        c.op("act", lambda e: e.copy(out=Ug[:, 16 * gh:16 * (gh + 1), :],
                                                     in_=pb[:, :].rearrange("p (g c) -> p g c", c=CG)),
                             reads=[bpb], writes=[b_Ug])
                    for gq in range(4):
                        pb, bpb = psum()
                        for gi in range(8):
                            g = 8 * gq + gi
                            for ri in range(2):
                                col = (gi * 2 + ri) * CG
                                c.op("pe", lambda e: e.matmul(pb[0:64, col:col + CG], lhsT=WinT[:, g, ri, :],
                                                              rhs=Ug[:, g, :], start=True, stop=True),
                                     reads=[b_WinT, b_Ug], writes=[bpb])
                        c.op("act", lambda e: e.copy(out=Zb[:, :, 8 * gq:8 * (gq + 1), :],
                                                     in_=pb[0:64, :].rearrange("p (g r c) -> p r g c", r=2, c=CG)),
                             reads=[bpb], writes=[b_Zb])
                    zre, zim = Zb[:, 0, :, :], Zb[:, 1, :, :]
                    rre, rim = Zr[:, 0, :, :], Zr[:, 1, :, :]
                    tre, tim = Tt[:, 0, :, :], Tt[:, 1, :, :]
                    c.op("dve", lambda e: d2(e, rre, zre, cosL[:], ALU.mult), reads=[b_Zb, b_tabL], writes=[b_Zr])
                    c.op("dve", lambda e: d2(e, tre, zim, sinL[:], ALU.mult), reads=[b_Zb, b_tabL], writes=[b_Tt])
                    c.op("dve", lambda e: d2(e, rre, rre, tre, ALU.add), reads=[b_Zr, b_Tt], writes=[b_Zr])
                    c.op("dve", lambda e: d2(e, rim, zim, cosL[:], ALU.mult), reads=[b_Zb, b_tabL], writes=[b_Zr])
                    c.op("dve", lambda e: d2(e, tre, zre, sinL[:], ALU.mult), reads=[b_Zb, b_tabL, b_Zr], writes=[b_Tt])
                    c.op("dve", lambda e: d2(e, rim, rim, tre, ALU.subtract), reads=[b_Zr, b_Tt], writes=[b_Zr])
                    c.op("dve", lambda e: d2(e, Zr[:, :, :, 0], Zr[:, :, :, 0], carry[:], ALU.add),
                         reads=[b_Zr, b_carry], writes=[b_Zr])
                    for ri in range(2):
                        c.op("dve", lambda e: e.tensor_tensor_scan(
                            out=Zr[:, ri, :, :].rearrange("p g c -> p (g c)"),
                            data0=rtab[:].rearrange("p g c -> p (g c)"),
                            data1=Zr[:, ri, :, :].rearrange("p g c -> p (g c)"),
                            initial=0.0, op0=ALU.mult, op1=ALU.add),
                             reads=[b_Zr, b_tabL], writes=[b_Zr])
                    c.op("dve", lambda e: d2(e, tre, rre, cosL[:], ALU.mult), reads=[b_Zr, b_tabL], writes=[b_Tt])
                    c.op("dve", lambda e: d2(e, tim, rim, sinL[:], ALU.mult), reads=[b_Zr, b_tabL], writes=[b_Tt])
                    c.op("dve", lambda e: d2(e, tre, tre, tim, ALU.subtract), reads=[b_Tt], writes=[b_Tt])
                    c.op("dve", lambda e: d2(e, tim, rim, cosL[:], ALU.mult), reads=[b_Zr, b_tabL], writes=[b_Tt])
                    c.op("dve", lambda e: d2(e, rim, rre, sinL[:], ALU.mult), reads=[b_Zr, b_tabL], writes=[b_Zr])
                    c.op("dve", lambda e: d2(e, tim, tim, rim, ALU.add), reads=[b_Tt, b_Zr], writes=[b_Tt])
                    c.op("dve", lambda e: d2(e, Rb[:], Tt[:], Zb[:], ALU.subtract), reads=[b_Tt, b_Zb], writes=[b_Rb])
                    tl = Tt[:, :, :, CG - 1]
                    m1 = Zr[:, :, :, 0]
                    m2 = Zr[:, :, :, 1]
                    c.op("dve", lambda e: d2(e, m1, A1[:], tl, ALU.mult), reads=[b_Tt, b_A], writes=[b_Zr])
                    c.op("dve", lambda e: d2(e, m2[:, 0, :], A2[:, 0, :], tl[:, 1, :], ALU.mult), reads=[b_Tt, b_A], writes=[b_Zr])
                    c.op("dve", lambda e: d2(e, m2[:, 1, :], A2[:, 1, :], tl[:, 0, :], ALU.mult), reads=[b_Tt, b_A], writes=[b_Zr])
                    c.op("dve", lambda e: d2(e, carry[:], m1, m2, ALU.add), reads=[b_Zr], writes=[b_carry])
                    for gh in range(2):
                        pb, bpb = psum()
                        for gi in range(16):
                            g = 16 * gh + gi
                            cs = slice(CG * gi, CG * (gi + 1))
                            c.op("pe", lambda e: e.matmul(pb[:, cs], lhsT=Toep[:, g, :], rhs=Ug[:, g, :], start=True, stop=False),
                                 reads=[b_Toep, b_Ug], writes=[bpb])
                            c.op("pe", lambda e: e.matmul(pb[:, cs], lhsT=Wout[:, 0, g, :], rhs=Rb[:, 0, g, :], start=False, stop=False),
                                 reads=[b_Wout, b_Rb], writes=[bpb])
                            c.op("pe", lambda e: e.matmul(pb[:, cs], lhsT=Wout[:, 1, g, :], rhs=Rb[:, 1, g, :], start=False, stop=True),
                                 reads=[b_Wout, b_Rb], writes=[bpb])
                        xx, bxx = tmp()
                        x2, bx2 = tmp()
                        c.op("act", lambda e: e.copy(out=xx[:, :], in_=pb[:, :]), reads=[bpb], writes=[bxx])
                        c.op("dve", lambda e: d2(e, x2[:, :], xx[:, :], xx[:, :], ALU.mult), reads=[bxx], writes=[bx2])
                        c.op("dve", lambda e: e.tensor_scalar(out=x2[:, :], in0=x2[:, :], scalar1=0.044715, scalar2=1.0,
                                                              op0=ALU.mult, op1=ALU.add), reads=[bx2], writes=[bx2])
                        c.op("dve", lambda e: d2(e, x2[:, :], x2[:, :], xx[:, :], ALU.mult), reads=[bx2, bxx], writes=[bx2])
                        c.op("act", lambda e: e.activation(out=x2[:, :], in_=x2[:, :], func=AF.Sigmoid,
                                                           scale=2.0 * math.sqrt(2.0 / math.pi)),
                             reads=[bx2], writes=[bx2])
                        c.op("dve", lambda e: d2(e, yg[:, 16 * gh:16 * (gh + 1), :],
                                                 x2[:, :].rearrange("p (g c) -> p g c", c=CG),
                                                 xx[:, :].rearrange("p (g c) -> p g c", c=CG), ALU.mult),
                             reads=[bx2, bxx], writes=[b_yg])
                    for cth in range(2):
                        pb, bpb = psum()
                        for cti in range(2):
                            ct = 2 * cth + cti
                            for tau in range(SK):
                                col = cti * GT + tau * CG
                                for j in range(8):
                                    o0 = 16 * (7 + 15 * tau - j)
                                    c.op("pe", lambda e: e.matmul(pb[:, col:col + CG], lhsT=tsel[:, o0:o0 + 128],
                                                                  rhs=yg[:, 8 * ct + j, :], start=(j == 0), stop=(j == 7)),
                                         reads=[b_yg, bconst], writes=[bpb])
                            c.op("act", lambda e: e.copy(out=ygT[:, ct, :].rearrange("p (c s) -> p s c", s=SK),
                                                         in_=pb[:, cti * GT:(cti + 1) * GT].rearrange("p (s c) -> p s c", s=SK)),
                                 reads=[bpb], writes=[b_ygT])
                    gw1, bgw1 = wload(wglu_v[:, :, 0:512], [128, 4, 512])
                    gw2, bgw2 = wload(wglu_v[:, :, 512:1024], [128, 4, 512])
                    for b_ in range(4):
                        p1, bp1 = psum()
                        for k in range(4):
                            c.op("pe", lambda e: e.matmul(p1[:, 0:GT], lhsT=gw1[:, k, 128 * b_:128 * (b_ + 1)], rhs=ygT[:, k, :],
                                                          start=(k == 0), stop=(k == 3)), reads=[bgw1, b_ygT], writes=[bp1])
                        p2, bp2 = psum()
                        for k in range(4):
                            c.op("pe", lambda e: e.matmul(p2[:, 0:GT], lhsT=gw2[:, k, 128 * b_:128 * (b_ + 1)], rhs=ygT[:, k, :],
                                                          start=(k == 0), stop=(k == 3)), reads=[bgw2, b_ygT], writes=[bp2])
                        sg2, bsg2 = tmp()
                        c.op("act", lambda e: e.activation(out=sg2[:, 0:GT], in_=p2[:, 0:GT], func=AF.Sigmoid),
                             reads=[bp2], writes=[bsg2])
                        c.op("dve", lambda e: d2(e, zgT[:, b_, :], sg2[:, 0:GT], p1[:, 0:GT], ALU.mult),
                             reads=[bsg2, bp1], writes=[b_zgT])

                for br in range(3):
                    for dq in range(2):
                        gw, bgw = wload(win_v[:, :, 2048 + 1024 * br + 512 * dq: 2048 + 1024 * br + 512 * (dq + 1)],
                                        [128, 8, 512])
                        if br == 0:
                            ow, bow = wload(wosw_v[:, :, 512 * dq:512 * (dq + 1)], [64, 8, 512])
                            osrc, bos, nk = oTa, b_oTa, 8
                        elif br == 1:
                            ow, bow = wload(wossm_v[:, :, 512 * dq:512 * (dq + 1)], [128, 4, 512])
                            osrc, bos, nk = zgT, b_zgT, 4
                        else:
                            ow, bow = wload(womb_v[:, :, 512 * dq:512 * (dq + 1)], [64, 8, 512])
                            osrc, bos, nk = oTm, b_oTm, 8
                        for j in range(4):
                            dt = 4 * dq + j
                            pg, bpg = psum()
                            for k in range(KD):
                                c.op("pe", lambda e: e.matmul(pg[:, 0:GT], lhsT=gw[:, k, 128 * j:128 * (j + 1)],
                                                              rhs=hT[:, k, :], start=(k == 0), stop=(k == KD - 1)),
                                     reads=[bgw, b_hT], writes=[bpg])
                            sg, bsg = tmp()
                            c.op("act", lambda e: e.activation(out=sg[:, 0:GT], in_=pg[:, 0:GT], func=AF.Sigmoid),
                                 reads=[bpg], writes=[bsg])
                            py, bpy = psum()
                            for k in range(nk):
                                c.op("pe", lambda e: e.matmul(py[:, 0:GT], lhsT=ow[:, k, 128 * j:128 * (j + 1)],
                                                              rhs=osrc[:, k, :], start=(k == 0), stop=(k == nk - 1)),
                                     reads=[bow, bos], writes=[bpy])
                            if br == 0:
                                c.op("dve", lambda e: e.tensor_tensor(out=macc[:, dt, :], in0=sg[:, 0:GT],
                                                                      in1=py[:, 0:GT], op=ALU.mult),
                                     reads=[bsg, bpy], writes=[b_macc])
                            else:
                                c.op("dve", lambda e: e.tensor_tensor(out=sg[:, 0:GT], in0=sg[:, 0:GT],
                                                                      in1=py[:, 0:GT], op=ALU.mult),
                                     reads=[bsg, bpy], writes=[bsg])
                                if br == 1:
                                    c.op("dve", lambda e: e.tensor_tensor(out=macc[:, dt, :], in0=macc[:, dt, :],
                                                                          in1=sg[:, 0:GT], op=ALU.add),
                                         reads=[bsg, b_macc], writes=[b_macc])
                                else:
                                    c.op("dve", lambda e: e.tensor_tensor(out=mixT[:, dt, :], in0=macc[:, dt, :],
                                                                          in1=sg[:, 0:GT], op=ALU.add),
                                         reads=[bsg, b_macc], writes=[b_mixT])
                for ch in range(2):
                    ww, bww = wload(wout_v[:, :, 512 * ch:512 * (ch + 1)], [128, 8, 512])
                    for t in range(NTG):
                        pb, bpb = psum()
                        for k in range(KD):
                            c.op("pe", lambda e: e.matmul(pb[:, :], lhsT=mixT[:, k, 128 * t:128 * (t + 1)],
                                                          rhs=ww[:, k, :], start=(k == 0), stop=(k == KD - 1)),
                                 reads=[bww, b_mixT], writes=[bpb])
                        c.op("dve", lambda e: e.tensor_tensor(out=x1[:, t, 512 * ch:512 * (ch + 1)],
                                                              in0=x1[:, t, 512 * ch:512 * (ch + 1)],
                                                              in1=pb[:, :], op=ALU.add),
                             reads=[bpb, b_x1], writes=[b_x1])
                dump_x1(c, x1, b_x1, r0, "merge")
                rmsnorm_to_T(gain_ffn, b_gffn, hT, b_hT, NTG)
                for qd in range(4):
                    for half in range(2):
                        fw, bfw = wload(wff1_v[:, :, 1024 * qd + 512 * half:1024 * qd + 512 * (half + 1)],
                                        [128, 8, 512])
                        for j in range(4):
                            pb, bpb = psum()
                            for k in range(KD):
                                c.op("pe", lambda e: e.matmul(pb[:, 0:GT], lhsT=fw[:, k, 128 * j:128 * (j + 1)],
                                                              rhs=hT[:, k, :], start=(k == 0), stop=(k == KD - 1)),
                                     reads=[bfw, b_hT], writes=[bpb])
                            tr, btr = tmp()
                            c.op("act", lambda e: e.activation(out=tr[:, 0:GT], in_=pb[:, 0:GT], func=AF.Relu),
                                 reads=[bpb], writes=[btr])
                            c.op("dve", lambda e: e.tensor_tensor(out=hid[:, 4 * half + j, :], in0=tr[:, 0:GT],
                                                                  in1=tr[:, 0:GT], op=ALU.mult),
                                 reads=[btr], writes=[b_hid])
                    for ch in range(2):
                        fw2, bfw2 = wload(wff2_v[:, 8 * qd:8 * (qd + 1), 512 * ch:512 * (ch + 1)], [128, 8, 512])
                        for t in range(NTG):
                            pb, bpb = psum()
                            for k in range(8):
                                c.op("pe", lambda e: e.matmul(pb[:, :], lhsT=hid[:, k, 128 * t:128 * (t + 1)],
                                                              rhs=fw2[:, k, :], start=(k == 0), stop=(k == 7)),
                                     reads=[bfw2, b_hid], writes=[bpb])
                            c.op("dve", lambda e: e.tensor_tensor(out=x1[:, t, 512 * ch:512 * (ch + 1)],
                                                                  in0=x1[:, t, 512 * ch:512 * (ch + 1)],
                                                                  in1=pb[:, :], op=ALU.add),
                                 reads=[bpb, b_x1], writes=[b_x1])
                dump_x1(c, x1, b_x1, r0, "ffn")
                if l < L - 1:
                    c.dma("sp", xs[r0:r0 + GT, :].rearrange("(t p) d -> p t d", p=128), x1[:],
                          reads=[b_x1], writes=[b_xg[G]])
                else:
                    for t in range(NTG):
                        c.op("act", lambda e: e.activation(out=hb[t % 2][:], in_=x1[:, t, :], func=AF.Square,
                                                           accum_out=ss[:, t:t + 1]),
                             reads=[b_x1], writes=[b_hb[t % 2], b_ss])
                    c.op("act", lambda e: e.activation(out=rstd[:, 0:NTG], in_=ss[:, 0:NTG], func=AF.Sqrt,
                                                       scale=1.0 / D, bias=epsc[:]),
                         reads=[b_ss, bconst], writes=[b_rstd])
                    c.op("dve", lambda e: e.reciprocal(out=rstd[:, 0:NTG], in_=rstd[:, 0:NTG]),
                         reads=[b_rstd], writes=[b_rstd])
                    for t in range(NTG):
                        c.op("dve", lambda e: e.scalar_tensor_tensor(out=x1[:, t, :], in0=x1[:, t, :],
                                                                     scalar=rstd[:, t:t + 1], in1=gain_fin[:],
                                                                     op0=ALU.mult, op1=ALU.mult),
                             reads=[b_x1, b_rstd, b_gfin], writes=[b_x1])
                    c.dma("sp", out[r0:r0 + GT, :].rearrange("(t p) d -> p t d", p=128), x1[:],
                          reads=[b_x1], writes=[b_xg[G]])
            last_bufs = b_xg
        c.finish(list(last_bufs) + [b_dbg])
        nc._ninstr = c.nins
    return nc, consts


INPUT_ORDER = ["x", "norm_mix", "w_in", "sinks", "lam_re", "lam_im", "log_step", "b_re", "b_im", "c_re", "c_im",
               "d_skip", "w_glu", "w_o_swa", "w_o_ssm", "w_o_moba", "w_out", "norm_ffn", "w_ff1", "w_ff2",
               "norm_final"]

_CACHE = {}


def run(inputs, S, L, n_cores=8, flags=None, trace=False):
    key = (S, L, str(flags))
    if key not in _CACHE:
        _CACHE[key] = build(S, L, flags)
    nc, consts = _CACHE[key]
    B = inputs["x"].shape[0]
    in_maps = []
    for cid in range(n_cores):
        b = cid % B
        m = {}
        for k in INPUT_ORDER:
            v = np.asarray(inputs[k], np.float32)
            if k == "x":
                m[k] = np.ascontiguousarray(v[b])
            elif k == "norm_final":
                m[k] = np.ascontiguousarray(v.reshape(1, D))
            else:
                m[k] = np.ascontiguousarray(v)
        for k, v in consts.items():
            m["c_" + k] = v
        in_maps.append(m)
    res = run_bass_kernel_spmd(nc, in_maps, core_ids=list(range(n_cores)), trace=trace)
    outs = [res.results[cid]["out"] for cid in range(B)]
    if flags and flags.get("dbg"):
        res.dbg = np.stack([res.results[cid]["dbg"] for cid in range(B)], axis=0)
    return np.stack(outs, axis=0).astype(np.float32), res


def kernel(**inputs):
    x = inputs["x"]
    S = x.shape[1]
    L = inputs["w_in"].shape[0]
    out, _ = run(inputs, S, L, n_cores=x.shape[0])
    return out
```
